# Optimizing a Trainium2 kernel written in Bass

```python
import jax, jax.numpy as jnp
from jax import lax
import numpy as np

D_MODEL = 1024
BATCH = 4
SEQ = 8192
DEPTH = 4

CTX_LEN = 256
GRID_W = 64
EPS = 1e-6

FOURIER_GROUPS = 4
FOURIER_GD = 64
FOURIER_W = FOURIER_GROUPS * FOURIER_GD
ATT_HEADS = 8
ATT_KV_HEADS = 2
ATT_GROUP = ATT_HEADS // ATT_KV_HEADS
HEAD_DIM = 64
ATT_W = ATT_HEADS * HEAD_DIM
ATT_KV_W = ATT_KV_HEADS * HEAD_DIM
HG_HEADS = 4
HG_DK = 64
HG_DV = 64
HG_W = HG_HEADS * HG_DK
MIX_W = FOURIER_W + ATT_W + HG_W

OFF_FOURIER = 0
OFF_Q = OFF_FOURIER + FOURIER_W
OFF_K = OFF_Q + ATT_W
OFF_V = OFF_K + ATT_KV_W
OFF_FF = OFF_V + ATT_KV_W
OFF_FB = OFF_FF + HG_W
OFF_I = OFF_FB + HG_W
OFF_HQ = OFF_I + HG_W
OFF_G = OFF_HQ + HG_W
PROJ_W = OFF_G + HG_W

D_FF = -(-8 * D_MODEL // (3 * 256)) * 256
ROPE_THETA = 10000.0
Q_BLOCK = 128
HG_CHUNK = 64

kernel_name = "hybrid_fourier_gqa_hgrn2_dit_prefix"


def rmsnorm(x, gain=None):
    xf = x.astype(jnp.float32)
    y = xf * lax.rsqrt(jnp.mean(xf * xf, axis=-1, keepdims=True) + EPS)
    if gain is not None:
        y = y * gain.astype(jnp.float32)
    return y.astype(x.dtype)


def col(u, off, width, base=0):
    return u[..., off - base: off - base + width]


def axial_rope_tables(n):
    rows = n // GRID_W
    row = jnp.repeat(jnp.arange(rows), GRID_W).astype(jnp.float32)
    colp = jnp.tile(jnp.arange(GRID_W), rows).astype(jnp.float32)
    half = HEAD_DIM // 2
    freqs = ROPE_THETA ** (-jnp.arange(0, half, 2, dtype=jnp.float32) / half)
    ang_r = row[:, None] * freqs
    ang_c = colp[:, None] * freqs
    return (jnp.cos(ang_r), jnp.sin(ang_r), jnp.cos(ang_c), jnp.sin(ang_c))


def rope_axis(x, cos, sin):
    x1, x2 = jnp.split(x, 2, axis=-1)
    return jnp.concatenate([x1 * cos - x2 * sin, x1 * sin + x2 * cos], axis=-1)


def apply_axial_rope(x, tabs):
    cr, sr, cc, sc = tabs
    xf = x.astype(jnp.float32)
    xr, xc = jnp.split(xf, 2, axis=-1)
    return jnp.concatenate([rope_axis(xr, cr, sr), rope_axis(xc, cc, sc)], axis=-1).astype(x.dtype)


def fourier_mix(u, w):
    b, n, _ = u.shape
    z = u.astype(jnp.float32).reshape(b, n, FOURIER_GROUPS, FOURIER_GD)
    z = jnp.fft.fftn(z, axes=(1, 3), norm="ortho").real
    return z.reshape(b, n, FOURIER_W).astype(u.dtype) @ w


def attend(q, k, v):
    s = jnp.einsum('bkgqd,bksd->bkgqs', q, k).astype(jnp.float32) * (HEAD_DIM ** -0.5)
    p = jax.nn.softmax(s, axis=-1).astype(v.dtype)
    return jnp.einsum('bkgqs,bksd->bkgqd', p, v)


def attend_blocked(q, k, v):
    b, kh, g, n, d = q.shape
    nb = n // Q_BLOCK
    qb = jnp.moveaxis(q.reshape(b, kh, g, nb, Q_BLOCK, d), 3, 0)
    ob = lax.map(lambda qi: attend(qi, k, v), qb)
    return jnp.moveaxis(ob, 0, 3).reshape(b, kh, g, n, d)


def q_heads(u, gain):
    b, n, _ = u.shape
    return rmsnorm(u.reshape(b, n, ATT_HEADS, HEAD_DIM), gain).transpose(0, 2, 1, 3)


def kv_heads(u, gain=None):
    b, n, _ = u.shape
    return rmsnorm(u.reshape(b, n, ATT_KV_HEADS, HEAD_DIM), gain).transpose(0, 2, 1, 3) if gain is not None \
        else u.reshape(b, n, ATT_KV_HEADS, HEAD_DIM).transpose(0, 2, 1, 3)


def group_q(qh):
    b, h, n, d = qh.shape
    return qh.reshape(b, ATT_KV_HEADS, ATT_GROUP, n, d)


def ungroup(o):
    b, kh, g, n, d = o.shape
    return o.transpose(0, 3, 1, 2, 4).reshape(b, n, kh * g * d)


def attention_group(ux, uc, base_c, q_gain, k_gain, tabs, ctx_out):
    qx = group_q(apply_axial_rope(q_heads(col(ux, OFF_Q, ATT_W), q_gain), tabs))
    kx = apply_axial_rope(kv_heads(col(ux, OFF_K, ATT_KV_W), k_gain), tabs)
    vx = kv_heads(col(ux, OFF_V, ATT_KV_W))
    kc = kv_heads(col(uc, OFF_K, ATT_KV_W, base_c), k_gain)
    vc = kv_heads(col(uc, OFF_V, ATT_KV_W, base_c))
    k_all = jnp.concatenate([kc, kx], axis=2)
    v_all = jnp.concatenate([vc, vx], axis=2)
    out_x = ungroup(attend_blocked(qx, k_all, v_all))
    out_c = None
    if ctx_out:
        qc = group_q(q_heads(col(uc, OFF_Q, ATT_W, base_c), q_gain))
        out_c = ungroup(attend(qc, kc, vc))
    return out_x, out_c


def hgrn_scan(k, v, log_f, s0, q=None):
    b, h, n, dk = k.shape
    nc = n // HG_CHUNK
    with_out = q is not None

    def chunks(a):
        return jnp.moveaxis(a.reshape(b, h, nc, HG_CHUNK, a.shape[-1]), 2, 0)

    mask = jnp.tril(jnp.ones((HG_CHUNK, HG_CHUNK), dtype=bool))[:, :, None]

    def step(s, inp):
        kc, vc, lf = inp[0], inp[1], inp[2]
        cum = jnp.cumsum(lf, axis=2)
        last = cum[:, :, -1:, :]
        s_new = jnp.exp(last[:, :, 0, :])[..., None] * s + \
            jnp.einsum('bhsd,bhse->bhde', kc * jnp.exp(last - cum), vc)
        if not with_out:
            return s_new, None
        qc = inp[3]
        o_inter = jnp.einsum('bhtd,bhde->bhte', qc * jnp.exp(cum), s)
        diff = cum[:, :, :, None, :] - cum[:, :, None, :, :]
        decay = jnp.exp(jnp.where(mask, diff, -jnp.inf))
        att = jnp.einsum('bhtd,bhsd,bhtsd->bhts', qc, kc, decay)
        return s_new, o_inter + jnp.einsum('bhts,bhse->bhte', att, vc)

    xs = (chunks(k), chunks(v), chunks(log_f)) + ((chunks(q),) if with_out else ())
    s_fin, o = lax.scan(step, s0, xs)
    if with_out:
        o = jnp.moveaxis(o, 0, 2).reshape(b, h, n, v.shape[-1])
    return o, s_fin


def hg_heads(u):
    b, n, _ = u.shape
    return u.reshape(b, n, HG_HEADS, -1).transpose(0, 2, 1, 3).astype(jnp.float32)


def hg_gate(z, lb):
    lb = lb.reshape(HG_HEADS, 1, HG_DK)
    k = (1.0 - lb) * jax.nn.sigmoid(-z)
    log_f = jnp.log(lb + (1.0 - lb) * jax.nn.sigmoid(z))
    return k, log_f


def hg_readout(o, g, gain, dtype):
    b, h, n, dv = o.shape
    y = rmsnorm(o, gain).transpose(0, 2, 1, 3).reshape(b, n, h * dv)
    return (y * jax.nn.silu(g.astype(jnp.float32))).astype(dtype)


def hgrn_group(ux, uc, base_c, lb_dirs, gain, ctx_out):
    b = ux.shape[0]
    vx = hg_heads(col(ux, OFF_I, HG_W))
    qx = hg_heads(col(ux, OFF_HQ, HG_W))
    vc = hg_heads(col(uc, OFF_I, HG_W, base_c))
    qc = hg_heads(col(uc, OFF_HQ, HG_W, base_c)) if ctx_out else None
    s0 = jnp.zeros((b, HG_HEADS, HG_DK, HG_DV), jnp.float32)
    o_x = jnp.zeros_like(vx)
    o_c = jnp.zeros_like(vc) if ctx_out else None
    for d, (off, rev) in enumerate(((OFF_FF, False), (OFF_FB, True))):
        fl = (lambda a: jnp.flip(a, axis=2)) if rev else (lambda a: a)
        kx, lfx = hg_gate(hg_heads(col(ux, off, HG_W)), lb_dirs[d])
        kc, lfc = hg_gate(hg_heads(col(uc, off, HG_W, base_c)), lb_dirs[d])
        oc, s_ctx = hgrn_scan(fl(kc), fl(vc), fl(lfc), s0, fl(qc) if ctx_out else None)
        ox, _ = hgrn_scan(fl(kx), fl(vx), fl(lfx), s_ctx, fl(qx))
        o_x = o_x + fl(ox)
        if ctx_out:
            o_c = o_c + fl(oc)
    out_x = hg_readout(o_x, col(ux, OFF_G, HG_W), gain, ux.dtype)
    out_c = hg_readout(o_c, col(uc, OFF_G, HG_W, base_c), gain, uc.dtype) if ctx_out else None
    return out_x, out_c


def swiglu(h, wg, wu, wd):
    return (jax.nn.silu(h @ wg) * (h @ wu)) @ wd


def setup_inputs(seed: int = 0) -> dict:
    key = jax.random.key(seed)
    ks = jax.random.split(key, 18)
    nrm = jax.random.normal
    f32 = jnp.float32
    return {
        "x": nrm(ks[0], (BATCH, SEQ, D_MODEL), f32),
        "c": nrm(ks[1], (BATCH, D_MODEL), f32),
        "ctx": nrm(ks[2], (BATCH, CTX_LEN, D_MODEL), f32),
        "c_ctx": nrm(ks[3], (D_MODEL,), f32),
        "w_ada": nrm(ks[4], (DEPTH, D_MODEL, 6 * D_MODEL), f32) * D_MODEL ** -0.5,
        "b_ada": nrm(ks[5], (DEPTH, 6 * D_MODEL), f32) * 0.02,
        "w_in": nrm(ks[6], (DEPTH, D_MODEL, PROJ_W), f32) * D_MODEL ** -0.5,
        "w_four": nrm(ks[7], (DEPTH, FOURIER_W, FOURIER_W), f32) * FOURIER_W ** -0.5,
        "q_norm": 1.0 + 0.02 * nrm(ks[8], (DEPTH, HEAD_DIM), f32),
        "k_norm": 1.0 + 0.02 * nrm(ks[9], (DEPTH, HEAD_DIM), f32),
        "hg_lb_logits": 0.5 * nrm(ks[10], (2, DEPTH, HG_W), f32),
        "hg_norm": 1.0 + 0.02 * nrm(ks[11], (DEPTH, HG_DV), f32),
        "w_out": nrm(ks[12], (DEPTH, MIX_W, D_MODEL), f32) * MIX_W ** -0.5,
        "w_gate": nrm(ks[13], (DEPTH, D_MODEL, D_FF), f32) * D_MODEL ** -0.5,
        "w_up": nrm(ks[14], (DEPTH, D_MODEL, D_FF), f32) * D_MODEL ** -0.5,
        "w_down": nrm(ks[15], (DEPTH, D_FF, D_MODEL), f32) * D_FF ** -0.5,
        "final_norm": 1.0 + 0.02 * nrm(ks[16], (D_MODEL,), f32),
    }


def reference(x, c, ctx, c_ctx, w_ada, b_ada, w_in, w_four, q_norm, k_norm,
              hg_lb_logits, hg_norm, w_out, w_gate, w_up, w_down, final_norm):
    d = D_MODEL
    tabs = axial_rope_tables(x.shape[1])
    silu_c = jax.nn.silu(c)
    silu_cc = jax.nn.silu(c_ctx)
    lb_sm = jax.nn.softmax(hg_lb_logits.astype(jnp.float32), axis=1)
    lb_all = jnp.cumsum(lb_sm, axis=1) - lb_sm[:, :1]
    for l in range(DEPTH):
        ctx_out = l < DEPTH - 1
        mod = silu_c @ w_ada[l] + b_ada[l]
        sh_a, sc_a, g_a, sh_f, sc_f, g_f = jnp.split(mod[:, None, :], 6, axis=-1)
        n_c = 6 * d if ctx_out else 2 * d
        mc = jnp.split(silu_cc @ w_ada[l][:, :n_c] + b_ada[l][:n_c], n_c // d)
        hx = rmsnorm(x) * (1 + sc_a) + sh_a
        hc = rmsnorm(ctx) * (1 + mc[1]) + mc[0]
        ux = hx @ w_in[l]
        base_c = 0 if ctx_out else OFF_K
        uc = hc @ (w_in[l] if ctx_out else w_in[l][:, OFF_K:OFF_HQ])
        fx = fourier_mix(col(ux, OFF_FOURIER, FOURIER_W), w_four[l])
        ax, ac = attention_group(ux, uc, base_c, q_norm[l], k_norm[l], tabs, ctx_out)
        rx, rc = hgrn_group(ux, uc, base_c, lb_all[:, l], hg_norm[l], ctx_out)
        x = x + g_a * (jnp.concatenate([fx, ax, rx], axis=-1) @ w_out[l])
        x = x + g_f * swiglu(rmsnorm(x) * (1 + sc_f) + sh_f, w_gate[l], w_up[l], w_down[l])
        if ctx_out:
            fc = fourier_mix(col(uc, OFF_FOURIER, FOURIER_W), w_four[l])
            ctx = ctx + mc[2] * (jnp.concatenate([fc, ac, rc], axis=-1) @ w_out[l])
            ctx = ctx + mc[5] * swiglu(rmsnorm(ctx) * (1 + mc[4]) + mc[3], w_gate[l], w_up[l], w_down[l])
    return rmsnorm(x, final_norm)
```

```python
import os
import numpy as np
import ml_dtypes
import concourse.bass as bass
import concourse.mybir as mybir
from concourse.bass_utils import run_bass_kernel_spmd

F32 = mybir.dt.float32
BF16 = mybir.dt.bfloat16
U8 = mybir.dt.uint8
AF = mybir.ActivationFunctionType
ALU = mybir.AluOpType
AX = mybir.AxisListType

D = 1024
KC = 8
PROJ = 2304
DFF = 2816
FC = 22
CTX = 256
EPS = 1e-6
SAME_ENGINE_SYNC = True
ROLL_AT = 12000
SKIP = set(os.environ.get('KSKIP', '').split(','))


class Sched:
    CE = ("pe", "act", "dve", "pool")

    def __init__(self, nc, nd=8):
        self.nc = nc
        self.E = {"pe": nc.tensor, "act": nc.scalar, "dve": nc.vector,
                  "pool": nc.gpsimd, "sp": nc.sync}
        self.sem = {}
        self.cnt = {}
        self.nsem = 0
        for e in self.CE:
            self.sem[("c", e)] = self._newsem()
            self.cnt[e] = 0
        self.nd = nd
        self.dval = {}
        self.dnext = {}
        for q in ("sp", "pool"):
            self.dnext[q] = 0
            for i in range(nd):
                self.sem[("d", q, i)] = self._newsem()
                self.dval[("d", q, i)] = 0
        self.seen = {e: {} for e in self.E}
        self.lastw = {}
        self.readers = {}
        self.ninst = 0

    def _newsem(self):
        self.nsem += 1
        return self.nc.semaphore("sm%d" % self.nsem).__enter__()

    def _wait(self, e, key, val, force=False):
        if val <= 0:
            return
        if key[0] == "c" and key[1] == e and not force:
            if e == "pe" or not SAME_ENGINE_SYNC:
                return
        if self.seen[e].get(key, 0) >= val:
            return
        if key[0] == "c":
            assert val <= self.cnt[key[1]], ("wait on unsignaled", e, key, val)
        self.E[e].wait_ge(self.sem[key], val)
        self.seen[e][key] = val

    def _deps(self, e, r, w, force=False):
        for b in r:
            for k, v in self.lastw.get(b, {}).items():
                self._wait(e, k, v, force)
        for b in w:
            for k, v in self.lastw.get(b, {}).items():
                self._wait(e, k, v, force)
            for k, v in self.readers.get(b, {}).items():
                self._wait(e, k, v, force)

    def _record(self, key, val, r, w):
        for b in r:
            d = self.readers.setdefault(b, {})
            if d.get(key, 0) < val:
                d[key] = val
        for b in w:
            d = self.lastw.setdefault(b, {})
            if d.get(key, 0) < val:
                d[key] = val
            self.readers[b] = {}

    def op(self, e, fn, r=(), w=(), signal=True):
        px = [b for b in r if b == "psT" or (isinstance(b, tuple) and b[0] == "ps")]
        if px:
            w = list(w) + px
        self._deps(e, r, w)
        inst = fn(self.E[e])
        self.ninst += 1
        key = ("c", e)
        if signal:
            self.cnt[e] += 1
            inst.then_inc(self.sem[key], 1)
            val = self.cnt[e]
        else:
            val = self.cnt[e] + 1
        self._record(key, val, r, w)
        return inst

    def dma(self, q, out, in_, r=(), w=(), **kw):
        i = self.dnext[q]
        self.dnext[q] = (i + 1) % self.nd
        key = ("d", q, i)
        self._wait(q, key, self.dval[key])
        self._deps(q, r, w, force=True)
        inst = self.E[q].dma_start(out=out, in_=in_, **kw)
        self.ninst += 1
        self.dval[key] += 16
        inst.then_inc(self.sem[key], 16)
        self._record(key, self.dval[key], r, w)
        return inst

    def barrier(self):
        for e in self.E:
            for e2 in self.CE:
                if e2 != e:
                    self._wait(e, ("c", e2), self.cnt[e2])
                elif e != "pe":
                    self._wait(e, ("c", e2), self.cnt[e2], force=True)
            for key, v in self.dval.items():
                self._wait(e, key, v)
        self.lastw = {}
        self.readers = {}
        for e in self.CE:
            if self.cnt[e] > ROLL_AT:
                self.sem[("c", e)] = self._newsem()
                self.cnt[e] = 0
                for s in self.seen.values():
                    s.pop(("c", e), None)
        for key in list(self.dval):
            if self.dval[key] > ROLL_AT:
                self.sem[key] = self._newsem()
                self.dval[key] = 0
                for s in self.seen.values():
                    s.pop(key, None)

    def maybe_roll(self):
        if max(self.cnt.values()) > ROLL_AT or max(self.dval.values()) > ROLL_AT:
            self.barrier()


def _bf(a):
    return np.ascontiguousarray(a.astype(np.float32)).astype(ml_dtypes.bfloat16)


def make_tables(S):
    NA = S // 128
    ST = S + CTX
    t = {}
    n = np.arange(S)
    row = (n // 64).astype(np.float32)
    colp = (n % 64).astype(np.float32)
    freqs = (np.float32(10000.0) ** (-np.arange(0, 32, 2, dtype=np.float32) / np.float32(32))).astype(np.float32)
    ang = [row[:, None] * freqs[None, :], colp[:, None] * freqs[None, :]]
    C = np.ones((ST, 64), np.float32)
    Sn = np.zeros((ST, 64), np.float32)
    for b in range(2):
        c = np.cos(ang[b]).astype(np.float32)
        s = np.sin(ang[b]).astype(np.float32)
        C[:S, b * 32:b * 32 + 16] = c
        C[:S, b * 32 + 16:b * 32 + 32] = c
        Sn[:S, b * 32:b * 32 + 16] = -s
        Sn[:S, b * 32 + 16:b * 32 + 32] = s
    t["ropeC"] = C
    t["ropeS"] = Sn
    cc = np.arange(64)
    a64 = 2 * np.pi * np.outer(cc, cc) / 64.0
    bd = np.zeros((128, 256))
    for g in range(2):
        bd[g * 64:(g + 1) * 64, g * 64:(g + 1) * 64] = np.cos(a64)
        bd[g * 64:(g + 1) * 64, 128 + g * 64:128 + (g + 1) * 64] = np.sin(a64)
    t["bdcs"] = _bf(bd)
    aa = np.arange(NA)
    al = 2 * np.pi * np.outer(aa, aa) / NA
    r1 = np.zeros((2 * NA, 2 * NA))
    r1[:NA, :NA] = np.cos(al)
    r1[NA:, :NA] = -np.sin(al)
    r1[:NA, NA:] = -np.sin(al)
    r1[NA:, NA:] = -np.cos(al)
    t["r1"] = _bf(r1)
    p = np.arange(128)
    k = (np.arange(NA)[:, None] + NA * np.arange(128)[None, :])
    kp = (p[:, None, None] * k[None]) % S
    be = 2 * np.pi * kp / S
    nrm = 1.0 / np.sqrt(S * 64.0)
    t["mcs"] = _bf(np.stack([np.cos(be) * nrm, np.sin(be) * nrm], axis=1))
    nn = (np.arange(2)[None, :, None] * 128 + p[:, None, None])
    an = 2 * np.pi * ((nn * np.arange(256)[None, None, :]) % 256) / 256.0
    nc_ = 1.0 / np.sqrt(256 * 64.0)
    t["ctab"] = _bf(np.stack([np.cos(an) * nc_, -np.sin(an) * nc_], axis=2))
    t["ident"] = _bf(np.eye(128))
    s_i = np.arange(128)[:, None]
    t_i = np.arange(128)[None, :]
    same = (s_i // 32) == (t_i // 32)
    t["hmask"] = np.stack([(same & (s_i <= t_i)), (same & (s_i >= t_i))], axis=1).astype(np.uint8)
    return t


def build_program(S, L, dbg=(), nphases=None):
    NA = S // 128
    ST = S + CTX
    NB = ST // 128
    nc = bass.Bass("TRN2", target_bir_lowering=False)

    def din(name, shape, dt=F32):
        return nc.dram_tensor(name, list(shape), dt, kind="ExternalInput").ap()

    def dscr(name, shape, dt=F32):
        kind = "ExternalOutput" if name in dbg else "Internal"
        return nc.dram_tensor(name, list(shape), dt, kind=kind).ap()

    uniq = [0]

    class Pool_:
        def __init__(self):
            self.items = []

        def sb(self, name, shape, dt=F32):
            uniq[0] += 1
            cm = nc.sbuf_tensor("s%d_%s" % (uniq[0], name), list(shape), dt)
            t_ = cm.__enter__()
            self.items.append(cm)
            return t_

        def free(self):
            for cm in reversed(self.items):
                cm.__exit__(None, None, None)
            self.items = []

    xT_in = din("xT_in", [128, KC, S])
    ctxT_in = din("ctxT_in", [128, KC, CTX])
    cT = din("cT", [128, KC, 2])
    w_ada = din("w_ada", [L, D, 6 * D])
    b_adaT = din("b_adaT", [128, L, 48])
    w_in = din("w_in", [L, D, PROJ])
    w_four = din("w_four", [L, 256, 256])
    qk_gain = din("qk_gain", [L, 640])
    lblT = din("lblT", [128, 4, L])
    hg_gain = din("hg_gain", [L, 256])
    w_out = din("w_out", [L, D, D])
    w_gate = din("w_gate", [L, D, DFF])
    w_up = din("w_up", [L, D, DFF])
    w_down = din("w_down", [L, DFF, D])
    fnormT = din("fnormT", [128, KC])
    ropeC = din("ropeC", [ST, 64])
    ropeS = din("ropeS", [ST, 64])
    bdcs_d = din("bdcs", [128, 256], BF16)
    r1_d = din("r1", [2 * NA, 2 * NA], BF16)
    mcs_d = din("mcs", [128, 2, NA, 128], BF16)
    ctab_d = din("ctab", [128, 2, 2, 256], BF16)
    ident_d = din("ident", [128, 128], BF16)
    hmask_d = din("hmask", [128, 2, 128], U8)
    outT = nc.dram_tensor("outT", [128, KC, S], F32, kind="ExternalOutput").ap()

    xT = dscr("xT", [128, KC, ST])
    zf = dscr("zf", [ST, 512], BF16)
    QT = dscr("QT", [128, 4, ST], BF16)
    lfT = [dscr("lfT%d" % d, [128, 2, ST]) for d in range(2)]
    kT = [dscr("kT%d" % d, [128, 2, ST], BF16) for d in range(2)]
    hqT = dscr("hqT", [128, 2, ST], BF16)
    vg = dscr("vg", [ST, 512], BF16)
    of = dscr("of", [ST, 256])
    mixT = dscr("mixT", [128, KC, ST], BF16)
    KTd = dscr("KTd", [128, ST], BF16)
    Vd = dscr("Vd", [ST, 128], BF16)
    h2T = dscr("h2T", [128, KC, ST], BF16)

    Sd = Sched(nc)
    op = Sd.op
    dma = Sd.dma

    G = Pool_()
    ident = G.sb("ident", [128, 128], BF16)
    ones_bf = G.sb("ones_bf", [128, 128], BF16)
    ones_f = G.sb("ones_f", [128, 64])
    hmask = G.sb("hmask", [128, 2, 128], U8)
    rmask = G.sb("rmask", [128, 512])
    cmsk = G.sb("cmsk", [128, 2, 512], BF16)
    scT = G.sb("scT", [128, KC, 2])
    modx = G.sb("modx", [128, L, 48])
    modc = G.sb("modc", [128, L, 48])
    lbt = G.sb("lbt", [128, 4, L])
    omlt = G.sb("omlt", [128, 4, L])
    qkg = G.sb("qkg", [128, 640])
    hgg = G.sb("hgg", [128, 256])
    fnorm = G.sb("fnorm", [128, KC])
    attT = [[G.sb("attT%d_%d" % (d, h), [128, 128], BF16) for h in range(4)] for d in range(2)]
    psF = [nc.psum_tensor("psF%d" % i, [128, 512], F32).__enter__() for i in range(7)]
    psT = nc.psum_tensor("psT", [128, 1024], BF16).__enter__()
    pscnt = [0]

    def nextps(lo=0, hi=7):
        i = lo + pscnt[0] % (hi - lo)
        pscnt[0] += 1
        return i

    def mm(out, lhsT, rhs, start, stop, r, w, sig=None):
        return op("pe", lambda e: e.matmul(out, lhsT, rhs, start=start, stop=stop), r=r, w=w, signal=stop if sig is None else sig)

    def act(out, in_, func, r, w, **kw):
        return op("act", lambda e: e.activation(out=out, in_=in_, func=func, **kw), r=r, w=w)

    def tiles_of(l):
        tl = [(t0, min(512, S - t0), False) for t0 in range(0, S, 512)]
        tl.append((S, CTX, True))
        return tl

    def setup():
        P = Pool_()
        dma("sp", ident[:], ident_d[:, :], w=["ident"])
        dma("sp", hmask[:], hmask_d[:, :, :], w=["hmask"])
        dma("sp", fnorm[:], fnormT[:, :], w=["fnorm"])
        dma("sp", xT[:, :, 0:S], xT_in[:, :, :], w=["xT"])
        dma("sp", xT[:, :, S:ST], ctxT_in[:, :, :], w=["xT"])
        op("pool", lambda e: e.memset(ones_bf[:], 1.0), w=["ones_bf"])
        op("pool", lambda e: e.memset(ones_f[:], 1.0), w=["ones_f"])
        op("pool", lambda e: e.memset(rmask[:], 1.0), w=["rmask"])
        op("pool", lambda e: e.memset(cmsk[:], 0.0), w=["cmsk"])
        for m_ in range(2):
            op("pool", lambda e, m_=m_: e.memset(cmsk[:, m_, :].rearrange("p (c two t) -> p c two t", two=2, t=32)[:, :, m_, :], 1.0), w=["cmsk"])
        op("pool", lambda e: e.memset(rmask[:].rearrange("p (c t) -> p c t", t=32)[:, :, 0:1], 0.0), w=["rmask"])
        for d in range(2):
            for h in range(4):
                op("pool", lambda e, d=d, h=h: e.memset(attT[d][h][:], 0.0), w=[("attT", d, h)])
        cin = P.sb("cin", [128, KC, 2])
        dma("sp", cin[:], cT[:, :, :], w=["cin"])
        act(scT[:], cin[:], AF.Silu, r=["cin"], w=["scT"])
        lbl = P.sb("lbl", [128, 4, L])
        ex = P.sb("lb_ex", [128, 4, L])
        sm = P.sb("lb_sm", [128, 4])
        dma("sp", lbl[:], lblT[:, :, :], w=["lbl"])
        act(ex[:], lbl[:], AF.Exp, r=["lbl"], w=["ex"])
        op("dve", lambda e: e.tensor_reduce(out=sm[:], in_=ex[:], axis=AX.X, op=ALU.add), r=["ex"], w=["sm"])
        op("dve", lambda e: e.reciprocal(out=sm[:], in_=sm[:]), r=["sm"], w=["sm"])
        op("dve", lambda e: e.tensor_tensor(out=ex[:], in0=ex[:], in1=sm[:].unsqueeze(2).to_broadcast([128, 4, L]), op=ALU.mult), r=["ex", "sm"], w=["ex"])
        op("dve", lambda e: e.memset(lbt[:], 0.0), w=["lbt"])
        for l in range(1, L):
            op("dve", lambda e, l=l: e.tensor_tensor(out=lbt[:, :, l:l + 1], in0=lbt[:, :, l - 1:l], in1=ex[:, :, l:l + 1], op=ALU.add), r=["ex", "lbt"], w=["lbt"])
        op("dve", lambda e: e.tensor_scalar(out=omlt[:], in0=lbt[:], scalar1=-1.0, scalar2=1.0, op0=ALU.mult, op1=ALU.add), r=["lbt"], w=["omlt"])
        bT = P.sb("bT", [128, L, 48])
        dma("sp", bT[:], b_adaT[:, :, :], w=["bT"])
        wa = [P.sb("wa%d" % i, [128, KC, 768]) for i in range(2)]
        nb_ = 0
        for l in range(L):
            pm = psF[nextps()]
            for cb in range(8):
                wt = wa[nb_ % 2]
                nb_ += 1
                for kc in range(KC):
                    dma("sp", wt[:, kc, :], w_ada[l, kc * 128:(kc + 1) * 128, cb * 768:(cb + 1) * 768], w=[("wa", id(wt), kc)])
                for j in range(6):
                    jj = cb * 6 + j
                    for kc in range(KC):
                        mm(pm[:, jj * 2:jj * 2 + 2], wt[:, kc, j * 128:(j + 1) * 128], scT[:, kc, :], kc == 0, kc == KC - 1,
                           r=[("wa", id(wt), kc), "scT"], w=[("ps", id(pm))])
            pv = pm[:, 0:96].rearrange("p (j t) -> p j t", t=2)
            op("dve", lambda e, l=l, pv=pv: e.tensor_tensor(out=modx[:, l, :], in0=pv[:, :, 0], in1=bT[:, l, :], op=ALU.add), r=[("ps", id(pm)), "bT"], w=["modx"])
            op("dve", lambda e, l=l, pv=pv: e.tensor_tensor(out=modc[:, l, :], in0=pv[:, :, 1], in1=bT[:, l, :], op=ALU.add), r=[("ps", id(pm)), "bT"], w=["modc"])
            for m_ in (modx, modc):
                for wh in (1, 4):
                    op("dve", lambda e, m_=m_, wh=wh, l=l: e.tensor_scalar_add(out=m_[:, l, wh * 8:(wh + 1) * 8], in0=m_[:, l, wh * 8:(wh + 1) * 8], scalar1=1.0),
                       r=["modx", "modc"], w=["modx", "modc"])
        Sd.barrier()
        P.free()

    def norm_mod(xt, xk, W, sq, rs, tmp, hT, hk, scale_ap, shift_ap):
        pst = psF[nextps()]
        for kc in range(KC):
            act(sq[:, kc % 2, :W], xt[:, kc, :W], AF.Square, r=[xk], w=[("sq", kc % 2)])
            mm(pst[:, :W], ones_bf[:, :], sq[:, kc % 2, :W], kc == 0, kc == KC - 1, r=[("sq", kc % 2), "ones_bf"], w=[("ps", id(pst))], sig=True)
        act(rs[:, :W], pst[:, :W], AF.Sqrt, r=[("ps", id(pst))], w=["rs"], bias=EPS, scale=1.0 / D)
        op("dve", lambda e: e.reciprocal(out=rs[:, :W], in_=rs[:, :W]), r=["rs"], w=["rs"])
        for kc in range(KC):
            tm = tmp[kc % 2]
            op("pool", lambda e, kc=kc, tm=tm: e.tensor_tensor(out=tm[:, :W], in0=xt[:, kc, :W], in1=rs[:, :W], op=ALU.mult),
               r=[xk, "rs"], w=[("tmp", kc % 2)])
            if scale_ap is None:
                op("dve", lambda e, kc=kc, tm=tm: e.tensor_copy(out=hT[:, kc, :W], in_=tm[:, :W]), r=[("tmp", kc % 2)], w=[hk])
            else:
                op("dve", lambda e, kc=kc, tm=tm: e.tensor_scalar(out=hT[:, kc, :W], in0=tm[:, :W], scalar1=scale_ap(kc), scalar2=shift_ap(kc),
                                                                  op0=ALU.mult, op1=ALU.add), r=[("tmp", kc % 2), "modx", "modc"], w=[hk])

    def load_w_bf16(dst, dkey, src_rows_of, nk, ncols):
        for kc in range(nk):
            for c0 in range(0, ncols, 1024):
                c1 = min(ncols, c0 + 1024)
                dma("pool", dst[:, kc, c0:c1], src_rows_of(kc)[:, c0:c1], w=[(dkey, kc)])

    def phase_A(l):
        P = Pool_()
        w_sb = P.sb("w_sb", [128, KC, PROJ], BF16)
        bdcs = P.sb("bdcs", [128, 256], BF16)
        xt = [P.sb("xt%d" % i, [128, KC, 512]) for i in range(2)]
        sq = P.sb("sq", [128, 2, 512], BF16)
        rs = P.sb("rs", [128, 512])
        tmp = [P.sb("tmp%d" % i, [128, 512]) for i in range(2)]
        hT = [P.sb("hT%d" % i, [128, KC, 512], BF16) for i in range(2)]
        zT = P.sb("zT", [128, 2, 512], BF16)
        zf_sb = P.sb("zf_sb", [128, 4, 512], BF16)
        sqs = P.sb("sqs", [128, 640])
        ss10 = P.sb("ss10", [128, 10])
        qn = P.sb("qn", [128, 640])
        t1 = P.sb("t1", [128, 640])
        t2 = P.sb("t2", [128, 640])
        qr = P.sb("qr", [128, 640], BF16)
        rc = [P.sb("rc%d" % i, [128, 4, 64]) for i in range(2)]
        rsn = [P.sb("rsn%d" % i, [128, 4, 64]) for i in range(2)]
        QTs = P.sb("QTs", [128, 4, 512], BF16)
        sg = P.sb("sg", [128, 512])
        ft = P.sb("ft", [128, 512])
        lf_sb = P.sb("lf_sb", [128, 2, 512])
        k_sb = P.sb("k_sb", [128, 2, 512], BF16)
        hq_sb = P.sb("hq_sb", [128, 2, 512], BF16)
        vg_sb = P.sb("vg_sb", [128, 4, 512], BF16)

        KTst = P.sb("KTst", [128, 512], BF16)
        vst = P.sb("vst", [128, 4, 128], BF16)
        dma("sp", bdcs[:], bdcs_d[:, :], w=["bdcs"])
        dma("sp", qkg[:], qk_gain[l].partition_broadcast(128), w=["qkg"])
        load_w_bf16(w_sb, "w_in", lambda kc: w_in[l, kc * 128:(kc + 1) * 128, :], KC, PROJ)
        wk = [("w_in", kc) for kc in range(KC)]

        for ti, (t0, W, isctx) in enumerate(tiles_of(l)):
            b = ti % 2
            nsub = W // 128
            md = modc if isctx else modx
            xk, hk = ("xt", b), ("hT", b)
            dma("sp", xt[b][:, :, :W], xT[:, :, t0:t0 + W], r=[("xT", t0)], w=[xk])
            dma("sp", rc[b][:, :nsub, :], ropeC[t0:t0 + W, :].rearrange("(j p) d -> p j d", p=128), w=[("rc", b)])
            dma("sp", rsn[b][:, :nsub, :], ropeS[t0:t0 + W, :].rearrange("(j p) d -> p j d", p=128), w=[("rsn", b)])
            norm_mod(xt[b], xk, W, sq, rs, tmp, hT[b], hk,
                     lambda kc: md[:, l, 8 + kc:9 + kc], lambda kc: md[:, l, kc:kc + 1])
            h = hT[b]
            for j in range(2 if 'four' not in SKIP else 0):
                pz = psF[nextps()]
                for kc in range(KC):
                    mm(pz[:, :W], w_sb[:, kc, j * 128:(j + 1) * 128], h[:, kc, :W], kc == 0, kc == KC - 1, r=[hk, wk[kc]], w=[("ps", id(pz))])
                act(zT[:, j, :W], pz[:, :W], AF.Copy, r=[("ps", id(pz))], w=["zT"])
            for s in range(nsub if 'four' not in SKIP else 0):
                pz = psF[nextps()]
                for j in range(2):
                    mm(pz[:, j * 256:(j + 1) * 256], zT[:, j, s * 128:(s + 1) * 128], bdcs[:, :], True, True, r=["zT", "bdcs"], w=[("ps", id(pz))])
                op("dve", lambda e, s=s, pz=pz: e.tensor_copy(
                    out=zf_sb[:, s, :].rearrange("p (cs j m) -> p j cs m", cs=2, j=2),
                    in_=pz[:, :].rearrange("p (j cs m) -> p j cs m", j=2, cs=2)), r=[("ps", id(pz))], w=["zf_sb"])
            dma("pool", zf[t0:t0 + W, :].rearrange("(s p) c -> p s c", p=128), zf_sb[:, :nsub, :], r=["zf_sb"], w=["zf"])
            for s in range(nsub if 'att' not in SKIP else 0):
                blk = (t0 + s * 128) // 128
                pq = psF[nextps()]
                pkv = psF[nextps()]
                for kc in range(KC):
                    mm(pq[:, :], h[:, kc, s * 128:(s + 1) * 128], w_sb[:, kc, 256:768], kc == 0, kc == KC - 1, r=[hk, wk[kc]], w=[("ps", id(pq))])
                for kc in range(KC):
                    mm(pkv[:, 0:256], h[:, kc, s * 128:(s + 1) * 128], w_sb[:, kc, 768:1024], kc == 0, kc == KC - 1, r=[hk, wk[kc]], w=[("ps", id(pkv))])
                act(sqs[:, 0:512], pq[:, :], AF.Square, r=[("ps", id(pq))], w=["sqs"])
                act(sqs[:, 512:640], pkv[:, 0:128], AF.Square, r=[("ps", id(pkv))], w=["sqs"])
                op("dve", lambda e: e.tensor_reduce(out=ss10[:], in_=sqs[:].rearrange("p (h d) -> p h d", d=64), axis=AX.X, op=ALU.add), r=["sqs"], w=["ss10"])
                act(ss10[:], ss10[:], AF.Sqrt, r=["ss10"], w=["ss10"], bias=EPS, scale=1.0 / 64)
                op("dve", lambda e: e.reciprocal(out=ss10[:], in_=ss10[:]), r=["ss10"], w=["ss10"])
                op("dve", lambda e, pq=pq: e.tensor_tensor(out=qn[:, 0:512].rearrange("p (h d) -> p h d", d=64), in0=pq[:, :].rearrange("p (h d) -> p h d", d=64),
                                                           in1=ss10[:, 0:8].unsqueeze(2).to_broadcast([128, 8, 64]), op=ALU.mult), r=[("ps", id(pq)), "ss10"], w=["qn"])
                op("dve", lambda e, pkv=pkv: e.tensor_tensor(out=qn[:, 512:640].rearrange("p (h d) -> p h d", d=64), in0=pkv[:, 0:128].rearrange("p (h d) -> p h d", d=64),
                                                             in1=ss10[:, 8:10].unsqueeze(2).to_broadcast([128, 2, 64]), op=ALU.mult), r=[("ps", id(pkv)), "ss10"], w=["qn"])
                act(vst[:, s, :], pkv[:, 128:256], AF.Copy, r=[("ps", id(pkv))], w=["vst"])
                if 'att_rope' in SKIP:
                    continue
                op("pool", lambda e: e.tensor_tensor(out=qn[:], in0=qn[:], in1=qkg[:, :], op=ALU.mult), r=["qn", "qkg"], w=["qn"])
                q3 = qn[:].rearrange("p (h d) -> p h d", d=64)
                op("dve", lambda e, s=s: e.tensor_tensor(out=t1[:].rearrange("p (h d) -> p h d", d=64), in0=q3,
                                                         in1=rc[b][:, s, :].unsqueeze(1).to_broadcast([128, 10, 64]), op=ALU.mult), r=["qn", ("rc", b)], w=["t1"])
                t23 = t2[:].rearrange("p (h d) -> p h d", d=64)
                for bb in range(2):
                    for f in range(2):
                        o_ = bb * 32 + f * 16
                        i_ = bb * 32 + (1 - f) * 16
                        op("pool", lambda e, s=s, o_=o_, i_=i_: e.tensor_tensor(out=t23[:, :, o_:o_ + 16], in0=q3[:, :, i_:i_ + 16],
                                                                              in1=rsn[b][:, s, o_:o_ + 16].unsqueeze(1).to_broadcast([128, 10, 16]), op=ALU.mult),
                           r=["qn", ("rsn", b)], w=["t2"])
                op("dve", lambda e: e.tensor_tensor(out=qr[:, 0:512].rearrange("p (j g d) -> p g j d", g=2, j=4),
                                                    in0=t1[:, 0:512].rearrange("p (g j d) -> p g j d", g=2, j=4),
                                                    in1=t2[:, 0:512].rearrange("p (g j d) -> p g j d", g=2, j=4), op=ALU.add), r=["t1", "t2"], w=["qr"])
                op("dve", lambda e: e.tensor_tensor(out=qr[:, 512:640], in0=t1[:, 512:640], in1=t2[:, 512:640], op=ALU.add), r=["t1", "t2"], w=["qr"])
                if 'att_tr' in SKIP:
                    continue
                for j in range(4):
                    op("pe", lambda e, j=j: e.transpose(psT[:, j * 128:(j + 1) * 128], qr[:, j * 128:(j + 1) * 128], ident[:, :]), r=["qr", "ident"], w=["psT"])
                op("pe", lambda e: e.transpose(psT[:, 512:640], qr[:, 512:640], ident[:, :]), r=["qr", "ident"], w=["psT"])
                act(QTs[:, :, s * 128:(s + 1) * 128], psT[:, 0:512].rearrange("p (j t) -> p j t", j=4), AF.Copy, r=["psT"], w=["QTs"])
                act(KTst[:, s * 128:(s + 1) * 128], psT[:, 512:640], AF.Copy, r=["psT"], w=["KTst"])
            dma("pool", QT[:, :, t0:t0 + W], QTs[:, :, :W], r=["QTs"], w=["QT"])
            dma("pool", KTd[:, t0:t0 + W], KTst[:, :W], r=["KTst"], w=["KTd"])
            dma("pool", Vd[t0:t0 + W, :].rearrange("(s p) c -> p s c", p=128), vst[:, :nsub, :], r=["vst"], w=["Vd"])
            for d in range(2 if 'hg' not in SKIP else 0):
                for hp in range(2):
                    c0 = 1024 + d * 256 + hp * 128
                    pg = psF[nextps()]
                    for kc in range(KC):
                        mm(pg[:, :W], w_sb[:, kc, c0:c0 + 128], h[:, kc, :W], kc == 0, kc == KC - 1, r=[hk, wk[kc]], w=[("ps", id(pg))])
                    act(sg[:, :W], pg[:, :W], AF.Sigmoid, r=[("ps", id(pg))], w=["sg"])
                    ix = d * 2 + hp
                    op("dve", lambda e, ix=ix: e.tensor_scalar(out=ft[:, :W], in0=sg[:, :W], scalar1=omlt[:, ix, l:l + 1], scalar2=lbt[:, ix, l:l + 1],
                                                               op0=ALU.mult, op1=ALU.add), r=["sg", "omlt", "lbt"], w=["ft"])
                    act(lf_sb[:, hp, :W], ft[:, :W], AF.Ln, r=["ft"], w=["lf_sb"])
                    op("pool", lambda e, d=d, hp=hp: e.tensor_scalar(out=k_sb[:, hp, :W], in0=ft[:, :W], scalar1=-1.0, scalar2=1.0, op0=ALU.mult, op1=ALU.add),
                       r=["ft"], w=["k_sb"])
                dma("pool", lfT[d][:, :, t0:t0 + W], lf_sb[:, :, :W], r=["lf_sb"], w=["lfT"])
                dma("pool", kT[d][:, :, t0:t0 + W], k_sb[:, :, :W], r=["k_sb"], w=["kT"])
            for hp in range(2):
                c0 = 1792 + hp * 128
                pg = psF[nextps()]
                for kc in range(KC):
                    mm(pg[:, :W], w_sb[:, kc, c0:c0 + 128], h[:, kc, :W], kc == 0, kc == KC - 1, r=[hk, wk[kc]], w=[("ps", id(pg))])
                act(hq_sb[:, hp, :W], pg[:, :W], AF.Copy, r=[("ps", id(pg))], w=["hq_sb"])
            dma("pool", hqT[:, :, t0:t0 + W], hq_sb[:, :, :W], r=["hq_sb"], w=["hqT"])
            for s in range(nsub if 'vg' not in SKIP else 0):
                pv = psF[nextps()]
                for kc in range(KC):
                    mm(pv[:, 0:256], h[:, kc, s * 128:(s + 1) * 128], w_sb[:, kc, 1536:1792], kc == 0, kc == KC - 1, r=[hk, wk[kc]], w=[("ps", id(pv))])
                for kc in range(KC):
                    mm(pv[:, 256:512], h[:, kc, s * 128:(s + 1) * 128], w_sb[:, kc, 2048:2304], kc == 0, kc == KC - 1, r=[hk, wk[kc]], w=[("ps", id(pv))])
                act(vg_sb[:, s, :], pv[:, :], AF.Copy, r=[("ps", id(pv))], w=["vg_sb"])
            dma("pool", vg[t0:t0 + W, :].rearrange("(s p) c -> p s c", p=128), vg_sb[:, :nsub, :], r=["vg_sb"], w=["vg"])
        Sd.barrier()
        P.free()

    def phase_F(l, ctx_out):
        P = Pool_()
        L1 = P.sb("L1", [2 * NA, 128, 128], BF16)
        PQ = P.sb("PQ", [128, 128, 2 * NA], BF16)
        mcs = P.sb("mcs", [128, 2, NA, 128], BF16)
        ZT = P.sb("ZT", [128, S], BF16)
        r1 = P.sb("r1", [2 * NA, 2 * NA], BF16)
        zfc = P.sb("zfc", [128, 2, 512], BF16)
        ctab = P.sb("ctab", [128, 2, 2, 256], BF16)
        ZTc = P.sb("ZTc", [128, 2, 256], BF16)
        dma("sp", r1[:], r1_d[:, :], w=["r1"])
        dma("sp", mcs[:], mcs_d[:, :, :, :], w=["mcs"])
        dma("sp", ctab[:], ctab_d[:, :, :, :], w=["ctab"])
        n4 = 0
        for j in range(2):
            for cs in range(2):
                src = zf[0:S, cs * 256 + j * 128:cs * 256 + (j + 1) * 128].rearrange("(a p) c -> a p c", p=128)
                for pb in range(0, 128, 32):
                    dma("sp", L1[cs * NA:(cs + 1) * NA, pb:pb + 32, :], src[:, pb:pb + 32, :], w=["L1"])
            for c4 in range(32):
                pp = psF[nextps()]
                for c in range(4):
                    ch = c4 * 4 + c
                    mm(pp[:, c * 2 * NA:(c + 1) * 2 * NA], L1[:, :, ch], r1[:, :], True, True, r=["L1", "r1"], w=[("ps", id(pp))])
                src_ = pp[:, 0:8 * NA].rearrange("p (c k) -> p c k", c=4)
                if n4 % 2 == 0:
                    act(PQ[:, c4 * 4:(c4 + 1) * 4, :], src_, AF.Copy, r=[("ps", id(pp))], w=["PQ"])
                else:
                    op("dve", lambda e, c4=c4, src_=src_: e.tensor_copy(out=PQ[:, c4 * 4:(c4 + 1) * 4, :], in_=src_), r=[("ps", id(pp))], w=["PQ"])
                n4 += 1
            ZTv = ZT[:].rearrange("c (k2 k1) -> c k1 k2", k1=NA)
            for k0 in range(0, NA, 4):
                pp = psF[nextps()]
                for i in range(4):
                    k1 = k0 + i
                    mm(pp[:, i * 128:(i + 1) * 128], PQ[:, :, k1], mcs[:, 0, k1, :], True, False, r=["PQ", "mcs"], w=[("ps", id(pp))])
                    mm(pp[:, i * 128:(i + 1) * 128], PQ[:, :, NA + k1], mcs[:, 1, k1, :], False, True, r=["PQ", "mcs"], w=[("ps", id(pp))])
                src_ = pp[:, :].rearrange("p (i k) -> p i k", i=4)
                if n4 % 2 == 0:
                    act(ZTv[:, k0:k0 + 4, :], src_, AF.Copy, r=[("ps", id(pp))], w=["ZT"])
                else:
                    op("dve", lambda e, k0=k0, src_=src_: e.tensor_copy(out=ZTv[:, k0:k0 + 4, :], in_=src_), r=[("ps", id(pp))], w=["ZT"])
                n4 += 1
            dma("pool", mixT[:, j, 0:S], ZT[:], r=["ZT"], w=["mixT"])
        if ctx_out:
            dma("sp", zfc[:], zf[S:ST, :].rearrange("(j p) c -> p j c", p=128), w=["zfc"])
            for j2 in range(2):
                pp = psF[nextps()]
                n_ = 0
                for nj in range(2):
                    for cs in range(2):
                        mm(pp[:, 0:256], zfc[:, nj, cs * 256 + j2 * 128:cs * 256 + (j2 + 1) * 128], ctab[:, nj, cs, :], n_ == 0, n_ == 3,
                           r=["zfc", "ctab"], w=[("ps", id(pp))])
                        n_ += 1
                act(ZTc[:, j2, :], pp[:, 0:256], AF.Copy, r=[("ps", id(pp))], w=["ZTc"])
            dma("pool", mixT[:, 0:2, S:ST], ZTc[:], r=["ZTc"], w=["mixT"])
        Sd.barrier()
        P.free()

    def phase_T(l, ctx_out):
        P = Pool_()
        QTt = [P.sb("QTt%d" % i, [128, 4, 256], BF16) for i in range(2)]
        pT = [P.sb("pT%d" % i, [128, 512], BF16) for i in range(3)]
        num = P.sb("num", [64, 512])
        rden = P.sb("rden", [128, 512])
        osb = [P.sb("osb%d" % i, [64, 512], BF16) for i in range(2)]
        KTs = P.sb("KTs", [128, ST], BF16)
        vaug = P.sb("vaug", [128, NB, 2, 66], BF16)
        op("pool", lambda e: e.memset(vaug[:], 1.0), w=["vaug"])
        dma("sp", KTs[:], KTd[:, :], w=["KTs"])
        for g in range(2):
            Vv = Vd[:, g * 64:(g + 1) * 64].rearrange("(b p) d -> p b d", p=128)
            for b0 in range(0, NB, 16):
                b1 = min(NB, b0 + 16)
                dma("sp", vaug[:, b0:b1, g, 0:64], Vv[:, b0:b1, :], r=["vaug"], w=["vaug"])
        qblocks = [(q0, list(range(NB))) for q0 in range(0, S, 256)]
        if ctx_out:
            qblocks.append((S, [NB - 2, NB - 1]))
        npt = 0
        ngrp = 0
        for qi, (q0, kbs) in enumerate(qblocks):
            qb = qi % 2
            dma("sp", QTt[qb][:], QT[:, :, q0:q0 + 256], r=["QT"], w=[("QTt", qb)])
            for g in range(2):
                for sp in range(2):
                    po = psF[4 + ngrp % 2]
                    for ki, kb in enumerate(kbs):
                        psc = psF[npt % 4]
                        pt_ = pT[npt % 3]
                        mm(psc[:, :], KTs[g * 64:(g + 1) * 64, kb * 128:(kb + 1) * 128], QTt[qb][g * 64:(g + 1) * 64, 2 * sp:2 * sp + 2, :], True, True,
                           r=["KTs", ("QTt", qb)], w=[("ps", id(psc))])
                        act(pt_[:, :], psc[:, :], AF.Exp, r=[("ps", id(psc))], w=[("pT", npt % 3)], scale=0.125)
                        mm(po[0:65, :], vaug[:, kb, g, 0:65], pt_[:, :], ki == 0, ki == len(kbs) - 1, r=["vaug", ("pT", npt % 3)], w=[("ps", id(po))])
                        npt += 1
                    ob = osb[ngrp % 2]
                    op("dve", lambda e, po=po: e.reciprocal(out=rden[64:65, :], in_=po[64:65, :]), r=[("ps", id(po))], w=["rden"])
                    pb = psF[6]
                    mm(pb[0:64, :], ones_f[64:65, 0:64], rden[64:65, :], True, True, r=["rden", "ones_f"], w=[("ps", id(pb))])
                    act(num[:, :], po[0:64, :], AF.Copy, r=[("ps", id(po))], w=["num"])
                    op("dve", lambda e, ob=ob, pb=pb: e.tensor_tensor(out=ob[:, :], in0=num[:, :], in1=pb[0:64, :], op=ALU.mult), r=["num", ("ps", id(pb))], w=[("osb", ngrp % 2)])
                    for jj in range(2):
                        hh = 4 * g + 2 * sp + jj
                        dma("pool", mixT[(hh % 2) * 64:(hh % 2 + 1) * 64, 2 + hh // 2, q0:q0 + 256], ob[:, jj * 256:(jj + 1) * 256], r=[("osb", ngrp % 2)], w=["mixT"])
                    ngrp += 1
            Sd.maybe_roll()
        Sd.barrier()
        P.free()

    def phase_H(l, ctx_out):
        P = Pool_()
        lf = [P.sb("lf%d" % i, [128, 2, 512]) for i in range(2)]
        kk = [P.sb("kk%d" % i, [128, 2, 512], BF16) for i in range(2)]
        qq = [P.sb("qq%d" % i, [128, 2, 512], BF16) for i in range(2)]
        vgt = [P.sb("vgt%d" % i, [128, 4, 512], BF16) for i in range(2)]
        cum = P.sb("cum", [128, 2, 512])
        a_t = P.sb("a_t", [128, 2, 512])
        ep = P.sb("ep", [128, 2, 512])
        em = P.sb("em", [128, 2, 512])
        qd = P.sb("qd", [128, 2, 512], BF16)
        kd = P.sb("kd", [128, 2, 512], BF16)
        dm = P.sb("dm", [128, 2, 16])
        dl = P.sb("dl", [128, 2, 16])
        dlm = P.sb("dlm", [128, 2, 16])
        kdT = P.sb("kdT", [128, 2, 2, 128], BF16)
        qdm = P.sb("qdm", [128, 2, 2, 512], BF16)
        kdm = P.sb("kdm", [128, 2, 2, 512], BF16)
        Sst = P.sb("Sst", [128, 2, 64])
        Sm = [P.sb("Sm%d" % i, [128, 2, 64], BF16) for i in range(4)]
        tmpU = P.sb("tmpU", [128, 2, 64])
        o_sb = [P.sb("o_sb%d" % i, [128, 256]) for i in range(2)]
        of_t = P.sb("of_t", [128, 256])
        osq = P.sb("osq", [128, 256])
        ss4 = P.sb("ss4", [128, 4])
        y_t = P.sb("y_t", [128, 256])
        sgl = P.sb("sgl", [128, 256])
        yb = P.sb("yb", [128, 256], BF16)
        yT = P.sb("yT", [128, 2, 512], BF16)
        rcum = P.sb("rcum", [128, 2, 512])
        dma("sp", hgg[:], hg_gain[l].partition_broadcast(128), w=["hgg"])
        nblk = 0
        nsm = 0
        nob = 0
        for d in range(2):
            op("dve", lambda e: e.memset(Sst[:], 0.0), r=["Sst"], w=["Sst"])
            lat = [(t0, min(512, S - t0)) for t0 in range(0, S, 512)]
            blocks = [(S, CTX, True)] + [(t0, W, False) for (t0, W) in (lat if d == 0 else lat[::-1])]
            mi, li = (15, 31) if d == 0 else (16, 0)
            for (t0, W, isctx) in blocks:
                b = nblk % 2
                nblk += 1
                nch = W // 32
                nsub = W // 128
                want_out = (not isctx) or ctx_out
                dma("sp", lf[b][:, :, :W], lfT[d][:, :, t0:t0 + W], r=["lfT"], w=[("lf", b)])
                dma("sp", kk[b][:, :, :W], kT[d][:, :, t0:t0 + W], r=["kT"], w=[("kk", b)])
                dma("sp", qq[b][:, :, :W], hqT[:, :, t0:t0 + W], r=["hqT"], w=[("qq", b)])
                dma("sp", vgt[b][:, :nsub, :], vg[t0:t0 + W, :].rearrange("(s p) c -> p s c", p=128), r=["vg"], w=[("vgt", b)])
                for hp in range(2):
                    op("dve", lambda e, hp=hp: e.tensor_tensor_scan(out=cum[:, hp, :W], data0=rmask[:, :W], data1=lf[b][:, hp, :W], initial=0.0,
                                                                    op0=ALU.mult, op1=ALU.add), r=["rmask", ("lf", b)], w=["cum"])
                c4 = cum[:, :, :W].rearrange("p h (c t) -> p h c t", t=32)
                ck = "cum"
                if d == 1:
                    op("dve", lambda e: e.tensor_tensor(out=a_t[:, :, :W], in0=lf[b][:, :, :W], in1=cum[:, :, :W], op=ALU.subtract), r=[("lf", b), "cum"], w=["a_t"])
                    op("dve", lambda e: e.tensor_tensor(out=rcum[:, :, :W].rearrange("p h (c t) -> p h c t", t=32), in0=a_t[:, :, :W].rearrange("p h (c t) -> p h c t", t=32),
                                                        in1=c4[:, :, :, 31:32].to_broadcast([128, 2, nch, 32]), op=ALU.add), r=["a_t", "cum"], w=["rcum"])
                    c4 = rcum[:, :, :W].rearrange("p h (c t) -> p h c t", t=32)
                    ck = "rcum"
                op("dve", lambda e: e.tensor_tensor(out=a_t[:, :, :W].rearrange("p h (c t) -> p h c t", t=32), in0=c4,
                                                    in1=c4[:, :, :, mi:mi + 1].to_broadcast([128, 2, nch, 32]), op=ALU.subtract), r=[ck], w=["a_t"])
                act(ep[:, :, :W], a_t[:, :, :W], AF.Exp, r=["a_t"], w=["ep"])
                act(em[:, :, :W], a_t[:, :, :W], AF.Exp, r=["a_t"], w=["em"], scale=-1.0)
                op("dve", lambda e: e.tensor_tensor(out=qd[:, :, :W], in0=qq[b][:, :, :W], in1=ep[:, :, :W], op=ALU.mult), r=[("qq", b), "ep"], w=["qd"])
                op("pool", lambda e: e.tensor_tensor(out=kd[:, :, :W], in0=kk[b][:, :, :W], in1=em[:, :, :W], op=ALU.mult), r=[("kk", b), "em"], w=["kd"])
                for m_ in range(2):
                    op("dve", lambda e, m_=m_: e.tensor_tensor(out=qdm[:, m_, :, :W], in0=qd[:, :, :W], in1=cmsk[:, m_, :W].unsqueeze(1).to_broadcast([128, 2, W]), op=ALU.mult),
                       r=["qd", "cmsk"], w=["qdm"])
                    op("pool", lambda e, m_=m_: e.tensor_tensor(out=kdm[:, m_, :, :W], in0=kd[:, :, :W], in1=cmsk[:, m_, :W].unsqueeze(1).to_broadcast([128, 2, W]), op=ALU.mult),
                       r=["kd", "cmsk"], w=["kdm"])
                act(dm[:, :, :nch], c4[:, :, :, mi], AF.Exp, r=[ck], w=["dm"])
                act(dl[:, :, :nch], c4[:, :, :, li], AF.Exp, r=[ck], w=["dl"])
                op("dve", lambda e: e.tensor_tensor(out=dlm[:, :, :nch], in0=c4[:, :, :, li], in1=c4[:, :, :, mi], op=ALU.subtract), r=[ck], w=["dlm"])
                act(dlm[:, :, :nch], dlm[:, :, :nch], AF.Exp, r=["dlm"], w=["dlm"])
                subs = list(range(nsub)) if d == 0 else list(range(nsub))[::-1]
                HL = int(os.environ.get("HLEVEL", "9"))
                if HL < 2:
                    subs = []
                for s in subs:
                    tk = slice(s * 128, (s + 1) * 128)
                    for m_ in range(2):
                        for hp in range(2):
                            op("pe", lambda e, hp=hp, m_=m_: e.transpose(psT[:, (m_ * 2 + hp) * 128:(m_ * 2 + hp + 1) * 128], kdm[:, m_, hp, tk], ident[:, :]), r=["kdm", "ident"], w=["psT"])
                    act(kdT[:].rearrange("p m h t -> p (m h) t"), psT[:, 0:512].rearrange("p (h t) -> p h t", h=4), AF.Copy, r=["psT"], w=["kdT"])
                    for h in range(4):
                        hp, par = h // 2, h % 2
                        pa = psF[par]
                        mm(pa[:, hp * 128:(hp + 1) * 128], kd[par * 64:(par + 1) * 64, hp, tk], qd[par * 64:(par + 1) * 64, hp, tk], True, True,
                           r=["kd", "qd"], w=[("ps", id(pa))])
                    for h in range(4 if 'h_cp' not in SKIP else 0):
                        pa = psF[h % 2]
                        op("dve", lambda e, h=h, pa=pa: e.copy_predicated(out=attT[d][h][:, :], mask=hmask[:, d, :], data=pa[:, (h // 2) * 128:(h // 2 + 1) * 128]),
                           r=[("ps", id(pa)), "hmask", ("attT", d, h)], w=[("attT", d, h)])
                    chunks = [0, 1, 2, 3] if d == 0 else [3, 2, 1, 0]
                    smb = {}
                    if HL < 3:
                        continue
                    for cp in chunks:
                        cg = s * 4 + cp
                        sb_ = nsm % 4
                        nsm += 1
                        smb[cp] = sb_
                        for hp in range(2):
                            op("act", lambda e, hp=hp, sb_=sb_, cg=cg: e.activation(out=Sm[sb_][:, hp, :], in_=Sst[:, hp, :], func=AF.Copy, scale=dm[:, hp, cg:cg + 1]),
                               r=["Sst", "dm"], w=[("Sm", sb_)])
                        pu = psF[4 + nsm % 2]
                        for h in range(4):
                            hp, par = h // 2, h % 2
                            mm(pu[par * 64:(par + 1) * 64, hp * 64:(hp + 1) * 64], kdT[(cp // 2) * 64:(cp // 2 + 1) * 64, cp % 2, hp, par * 64:(par + 1) * 64],
                               vgt[b][(cp // 2) * 64:(cp // 2 + 1) * 64, s, h * 64:(h + 1) * 64], True, True, r=["kdT", ("vgt", b)], w=[("ps", id(pu))])
                        for hp in range(2):
                            op("dve", lambda e, hp=hp, pu=pu, cg=cg: e.tensor_scalar(out=tmpU[:, hp, :], in0=pu[:, hp * 64:(hp + 1) * 64], scalar1=dlm[:, hp, cg:cg + 1], scalar2=None,
                                                                                    op0=ALU.mult), r=[("ps", id(pu)), "dlm"], w=["tmpU"])
                            op("dve", lambda e, hp=hp, cg=cg: e.scalar_tensor_tensor(out=Sst[:, hp, :], in0=Sst[:, hp, :], scalar=dl[:, hp, cg:cg + 1], in1=tmpU[:, hp, :],
                                                                                     op0=ALU.mult, op1=ALU.add), r=["Sst", "tmpU", "dl"], w=["Sst"])
                    if not want_out or HL < 4:
                        continue
                    for h in (0, 2, 1, 3):
                        hp, par = h // 2, h % 2
                        po = psF[2 + par]
                        mm(po[:, hp * 64:(hp + 1) * 64], attT[d][h][:, :], vgt[b][:, s, h * 64:(h + 1) * 64], True, False, r=[("attT", d, h), ("vgt", b)], w=[("ps", id(po))])
                        for ci, cp in enumerate(chunks):
                            mm(po[(cp // 2) * 64:(cp // 2 + 1) * 64, hp * 64:(hp + 1) * 64], qdm[par * 64:(par + 1) * 64, cp % 2, hp, s * 128 + (cp // 2) * 64:s * 128 + (cp // 2 + 1) * 64],
                               Sm[smb[cp]][par * 64:(par + 1) * 64, hp, :], False, ci % 2 == 1, r=["qdm", ("Sm", smb[cp])], w=[("ps", id(po))])
                    if HL < 5:
                        continue
                    ob = o_sb[nob % 2]
                    okey = ("o_sb", nob % 2)
                    nob += 1
                    r0 = t0 + s * 128
                    obv = ob[:, :].rearrange("p (hp par d) -> p hp par d", par=2, d=64)
                    if d == 0:
                        for par in range(2):
                            act(obv[:, :, par, :], psF[2 + par][:, 0:128].rearrange("p (hp d) -> p hp d", d=64), AF.Copy, r=[("ps", id(psF[2 + par]))], w=[okey])
                        dma("pool", of[r0:r0 + 128, :], ob[:, :], r=[okey], w=[("of", r0)])
                    else:
                        dma("sp", of_t[:, :], of[r0:r0 + 128, :], r=[("of", r0)], w=["of_t"])
                        for par in range(2):
                            op("dve", lambda e, par=par: e.tensor_tensor(out=obv[:, :, par, :], in0=psF[2 + par][:, 0:128].rearrange("p (hp d) -> p hp d", d=64),
                                                                         in1=of_t[:, :].rearrange("p (hp par d) -> p hp par d", par=2, d=64)[:, :, par, :], op=ALU.add),
                               r=[("ps", id(psF[2 + par])), "of_t"], w=[okey])
                        act(osq[:, :], ob[:, :], AF.Square, r=[okey], w=["osq"])
                        op("dve", lambda e: e.tensor_reduce(out=ss4[:], in_=osq[:].rearrange("p (h d) -> p h d", d=64), axis=AX.X, op=ALU.add), r=["osq"], w=["ss4"])
                        act(ss4[:], ss4[:], AF.Sqrt, r=["ss4"], w=["ss4"], bias=EPS, scale=1.0 / 64)
                        op("dve", lambda e: e.reciprocal(out=ss4[:], in_=ss4[:]), r=["ss4"], w=["ss4"])
                        op("dve", lambda e, ob=ob: e.tensor_tensor(out=y_t[:].rearrange("p (h d) -> p h d", d=64), in0=ob[:, :].rearrange("p (h d) -> p h d", d=64),
                                                                   in1=ss4[:].unsqueeze(2).to_broadcast([128, 4, 64]), op=ALU.mult), r=[okey, "ss4"], w=["y_t"])
                        op("pool", lambda e: e.tensor_tensor(out=y_t[:], in0=y_t[:], in1=hgg[:, :], op=ALU.mult), r=["y_t", "hgg"], w=["y_t"])
                        act(sgl[:, :], vgt[b][:, s, 256:512], AF.Silu, r=[("vgt", b)], w=["sgl"])
                        op("dve", lambda e: e.tensor_tensor(out=yb[:], in0=y_t[:], in1=sgl[:], op=ALU.mult), r=["y_t", "sgl"], w=["yb"])
                        for hp in range(2):
                            op("pe", lambda e, hp=hp: e.transpose(psT[:, (2 + hp) * 128:(3 + hp) * 128], yb[:, hp * 128:(hp + 1) * 128], ident[:, :]), r=["yb", "ident"], w=["psT"])
                        act(yT[:, :, tk], psT[:, 256:512].rearrange("p (h t) -> p h t", h=2), AF.Copy, r=["psT"], w=["yT"])
                if d == 1 and want_out and HL >= 5:
                    dma("pool", mixT[:, 6:8, t0:t0 + W], yT[:, :, :W], r=["yT"], w=["mixT"])
            Sd.maybe_roll()
        Sd.barrier()
        P.free()

    def phase_C1(l, ctx_out):
        P = Pool_()
        wfo = P.sb("wfo", [128, 2, 256], BF16)
        wo = P.sb("wo", [128, KC, D], BF16)
        mx = [P.sb("mx%d" % i, [128, KC, 512], BF16) for i in range(2)]
        xt = [P.sb("xt%d" % i, [128, KC, 512]) for i in range(2)]
        fx = P.sb("fx", [128, 2, 512], BF16)
        sq = P.sb("sq", [128, 2, 512], BF16)
        rs = P.sb("rs", [128, 512])
        tmp = [P.sb("tmp%d" % i, [128, 512]) for i in range(2)]
        h2 = [P.sb("h2_%d" % i, [128, KC, 512], BF16) for i in range(2)]
        load_w_bf16(wfo, "wfo", lambda kc: w_four[l, kc * 128:(kc + 1) * 128, :], 2, 256)
        load_w_bf16(wo, "wo", lambda kc: w_out[l, kc * 128:(kc + 1) * 128, :], KC, D)
        for ti, (t0, W, isctx) in enumerate(tiles_of(l)):
            if isctx and not ctx_out:
                continue
            b = ti % 2
            md = modc if isctx else modx
            xk, mk, hk = ("xt", b), ("mx", b), ("h2", b)
            dma("sp", mx[b][:, :, :W], mixT[:, :, t0:t0 + W], r=["mixT"], w=[mk])
            dma("sp", xt[b][:, :, :W], xT[:, :, t0:t0 + W], r=[("xT", t0)], w=[xk])
            for jo in range(2):
                pf = psF[nextps()]
                for ji in range(2):
                    mm(pf[:, :W], wfo[:, ji, jo * 128:(jo + 1) * 128], mx[b][:, ji, :W], ji == 0, ji == 1, r=[mk, ("wfo", ji)], w=[("ps", id(pf))])
                act(fx[:, jo, :W], pf[:, :W], AF.Copy, r=[("ps", id(pf))], w=["fx"])
            for oc in range(KC):
                pp = psF[nextps()]
                for kc in range(KC):
                    rhs = fx[:, kc, :W] if kc < 2 else mx[b][:, kc, :W]
                    mm(pp[:, :W], wo[:, kc, oc * 128:(oc + 1) * 128], rhs, kc == 0, kc == KC - 1, r=[mk, "fx", ("wo", kc)], w=[("ps", id(pp))])
                op("dve", lambda e, oc=oc, pp=pp: e.scalar_tensor_tensor(out=xt[b][:, oc, :W], in0=pp[:, :W], scalar=md[:, l, 16 + oc:17 + oc], in1=xt[b][:, oc, :W],
                                                                         op0=ALU.mult, op1=ALU.add), r=[("ps", id(pp)), xk, "modx", "modc"], w=[xk])
            dma("pool", xT[:, :, t0:t0 + W], xt[b][:, :, :W], r=[xk], w=[("xT", t0)])
            norm_mod(xt[b], xk, W, sq, rs, tmp, h2[b], hk, lambda kc: md[:, l, 32 + kc:33 + kc], lambda kc: md[:, l, 24 + kc:25 + kc])
            dma("pool", h2T[:, :, t0:t0 + W], h2[b][:, :, :W], r=[hk], w=["h2T"])
        Sd.barrier()
        P.free()

    def phase_C2(l, half, ctx_out, final):
        P = Pool_()
        HF = FC // 2
        wg = P.sb("wg", [128, KC, HF * 128], BF16)
        wu = P.sb("wu", [128, KC, HF * 128], BF16)
        wd = P.sb("wd", [128, HF, D], BF16)
        ht = [P.sb("ht%d" % i, [128, KC, 512], BF16) for i in range(2)]
        xt = [P.sb("xt%d" % i, [128, KC, 512]) for i in range(2)]
        a_sb = P.sb("a_sb", [128, HF, 512], BF16)
        sgt = [P.sb("sgt%d" % i, [128, 512]) for i in range(2)]
        sq = P.sb("sq", [128, 2, 512], BF16)
        rs = P.sb("rs", [128, 512])
        tmp = [P.sb("tmp%d" % i, [128, 512]) for i in range(2)]
        c0 = half * HF * 128
        load_w_bf16(wg, "wg", lambda kc: w_gate[l, kc * 128:(kc + 1) * 128, c0:c0 + HF * 128], KC, HF * 128)
        load_w_bf16(wu, "wu", lambda kc: w_up[l, kc * 128:(kc + 1) * 128, c0:c0 + HF * 128], KC, HF * 128)
        load_w_bf16(wd, "wd", lambda fc: w_down[l, c0 + fc * 128:c0 + (fc + 1) * 128, :], HF, D)
        for ti, (t0, W, isctx) in enumerate(tiles_of(l)):
            if isctx and not ctx_out:
                continue
            b = ti % 2
            md = modc if isctx else modx
            xk, hk = ("xt", b), ("ht", b)
            dma("sp", ht[b][:, :, :W], h2T[:, :, t0:t0 + W], r=["h2T"], w=[hk])
            dma("sp", xt[b][:, :, :W], xT[:, :, t0:t0 + W], r=[("xT", t0)], w=[xk])
            for fc in range(HF):
                pg = psF[nextps()]
                pu = psF[nextps()]
                for kc in range(KC):
                    mm(pg[:, :W], wg[:, kc, fc * 128:(fc + 1) * 128], ht[b][:, kc, :W], kc == 0, kc == KC - 1, r=[hk, ("wg", kc)], w=[("ps", id(pg))])
                for kc in range(KC):
                    mm(pu[:, :W], wu[:, kc, fc * 128:(fc + 1) * 128], ht[b][:, kc, :W], kc == 0, kc == KC - 1, r=[hk, ("wu", kc)], w=[("ps", id(pu))])
                st = sgt[fc % 2]
                act(st[:, :W], pg[:, :W], AF.Silu, r=[("ps", id(pg))], w=[("sgt", fc % 2)])
                op("dve", lambda e, fc=fc, st=st, pu=pu: e.tensor_tensor(out=a_sb[:, fc, :W], in0=st[:, :W], in1=pu[:, :W], op=ALU.mult),
                   r=[("sgt", fc % 2), ("ps", id(pu))], w=["a_sb"])
            for oc in range(KC):
                pp = psF[nextps()]
                for fc in range(HF):
                    mm(pp[:, :W], wd[:, fc, oc * 128:(oc + 1) * 128], a_sb[:, fc, :W], fc == 0, fc == HF - 1, r=["a_sb", ("wd", fc)], w=[("ps", id(pp))])
                op("dve", lambda e, oc=oc, pp=pp: e.scalar_tensor_tensor(out=xt[b][:, oc, :W], in0=pp[:, :W], scalar=md[:, l, 40 + oc:41 + oc], in1=xt[b][:, oc, :W],
                                                                         op0=ALU.mult, op1=ALU.add), r=[("ps", id(pp)), xk, "modx", "modc"], w=[xk])
            if final and not isctx:
                pst = psF[nextps()]
                for kc in range(KC):
                    act(sq[:, kc % 2, :W], xt[b][:, kc, :W], AF.Square, r=[xk], w=[("sq", kc % 2)])
                    mm(pst[:, :W], ones_bf[:, :], sq[:, kc % 2, :W], kc == 0, kc == KC - 1, r=[("sq", kc % 2), "ones_bf"], w=[("ps", id(pst))], sig=True)
                act(rs[:, :W], pst[:, :W], AF.Sqrt, r=[("ps", id(pst))], w=["rs"], bias=EPS, scale=1.0 / D)
                op("dve", lambda e: e.reciprocal(out=rs[:, :W], in_=rs[:, :W]), r=["rs"], w=["rs"])
                for kc in range(KC):
                    op("pool", lambda e, kc=kc: e.tensor_tensor(out=xt[b][:, kc, :W], in0=xt[b][:, kc, :W], in1=rs[:, :W], op=ALU.mult), r=[xk, "rs"], w=[xk])
                    op("dve", lambda e, kc=kc: e.tensor_scalar(out=xt[b][:, kc, :W], in0=xt[b][:, kc, :W], scalar1=fnorm[:, kc:kc + 1], scalar2=None, op0=ALU.mult),
                       r=[xk, "fnorm"], w=[xk])
                dma("pool", outT[:, :, t0:t0 + W], xt[b][:, :, :W], r=[xk], w=[("outT", t0)])
            else:
                dma("pool", xT[:, :, t0:t0 + W], xt[b][:, :, :W], r=[xk], w=[("xT", t0)])
        Sd.barrier()
        P.free()

    plan = [setup]
    for l in range(L):
        ctx_out = l < L - 1
        plan.append(lambda l=l: phase_A(l))
        plan.append(lambda l=l, c=ctx_out: phase_F(l, c))
        plan.append(lambda l=l, c=ctx_out: phase_T(l, c))
        plan.append(lambda l=l, c=ctx_out: phase_H(l, c))
        plan.append(lambda l=l, c=ctx_out: phase_C1(l, c))
        plan.append(lambda l=l, c=ctx_out: phase_C2(l, 0, c, False))
        plan.append(lambda l=l, c=ctx_out: phase_C2(l, 1, c, l == L - 1))
    for f in plan[:nphases]:
        f()
    Sd.barrier()
    return nc, Sd


def _fm(a):
    T_ = a.shape[0]
    return np.ascontiguousarray(a.reshape(T_, KC, 128).transpose(2, 1, 0))


def make_in_maps(inputs, S, L, n_cores, nbatch):
    f32 = np.float32
    x = np.asarray(inputs["x"], f32)
    c = np.asarray(inputs["c"], f32)
    ctx = np.asarray(inputs["ctx"], f32)
    c_ctx = np.asarray(inputs["c_ctx"], f32)
    tabs = make_tables(S)
    shared = {
        "w_ada": np.ascontiguousarray(np.asarray(inputs["w_ada"], f32)),
        "b_adaT": np.ascontiguousarray(np.asarray(inputs["b_ada"], f32).reshape(L, 48, 128).transpose(2, 0, 1)),
        "w_in": np.ascontiguousarray(np.asarray(inputs["w_in"], f32)),
        "w_four": np.ascontiguousarray(np.asarray(inputs["w_four"], f32)),
        "qk_gain": np.ascontiguousarray(np.concatenate([np.tile(np.asarray(inputs["q_norm"], f32), (1, 8)),
                                                        np.tile(np.asarray(inputs["k_norm"], f32), (1, 2))], axis=1)),
        "lblT": np.ascontiguousarray(np.asarray(inputs["hg_lb_logits"], f32).reshape(2, L, 2, 128).transpose(3, 0, 2, 1).reshape(128, 4, L)),
        "hg_gain": np.ascontiguousarray(np.tile(np.asarray(inputs["hg_norm"], f32), (1, 4))),
        "w_out": np.ascontiguousarray(np.asarray(inputs["w_out"], f32)),
        "w_gate": np.ascontiguousarray(np.asarray(inputs["w_gate"], f32)),
        "w_up": np.ascontiguousarray(np.asarray(inputs["w_up"], f32)),
        "w_down": np.ascontiguousarray(np.asarray(inputs["w_down"], f32)),
        "fnormT": np.ascontiguousarray(np.asarray(inputs["final_norm"], f32).reshape(KC, 128).T),
    }
    shared.update(tabs)
    maps = []
    for i in range(n_cores):
        b = i % nbatch
        m = dict(shared)
        m["xT_in"] = _fm(x[b])
        m["ctxT_in"] = _fm(ctx[b])
        m["cT"] = np.ascontiguousarray(np.stack([c[b].reshape(KC, 128).T, c_ctx.reshape(KC, 128).T], axis=2))
        maps.append(m)
    return maps


_PROG = {}


def kernel(**inputs):
    x = np.asarray(inputs["x"])
    B, S, _ = x.shape
    L = np.asarray(inputs["w_ada"]).shape[0]
    key = (S, L)
    if key not in _PROG:
        _PROG[key] = build_program(S, L)[0]
    nc = _PROG[key]
    n_cores = 8
    maps = make_in_maps(inputs, S, L, n_cores, B)
    res = run_bass_kernel_spmd(nc, maps, core_ids=list(range(n_cores)))
    out = np.empty((B, S, D), np.float32)
    for b in range(B):
        o = res.results[b]["outT"]
        out[b] = o.transpose(2, 1, 0).reshape(S, D)
    return out
```
